# Optimizing a Trainium2 kernel written in Bass

```python
import math
import jax
import jax.numpy as jnp
from jax import lax
import numpy as np

D_MODEL = 4096
BATCH = 2
SEQ = 4096
DEPTH = 2

GRID_W = 64
CTX_LEN = 256
N_Q_HEADS = 16
N_KV_HEADS = 4
HEAD_DIM = 128
Q_GROUP = N_Q_HEADS // N_KV_HEADS
ATT_WIDTH = N_Q_HEADS * HEAD_DIM
KV_WIDTH = N_KV_HEADS * HEAD_DIM
WINDOW = 128
BLOCK = 128
ROPE_BASE = 10000.0
ROPE_AXIS_DIM = HEAD_DIM // 2
ROPE_FREQS = ROPE_AXIS_DIM // 2
MASK_VALUE = -1e30
HY_WIDTH = 2048
HY_ORDER = 2
HY_DIRS = 2
SHORT_CONV = 3
FILTER_BANDS = 16
FILTER_EMB = 1 + 2 * FILTER_BANDS
FILTER_HIDDEN = 64
DECAY_TARGET = 1e-2
FAST_DECAY_PCT = 0.3
SLOW_DECAY_PCT = 1.5
MIN_DECAY = math.log(DECAY_TARGET) / SLOW_DECAY_PCT
MAX_DECAY = math.log(DECAY_TARGET) / FAST_DECAY_PCT
K_OFF = ATT_WIDTH
V_OFF = K_OFF + KV_WIDTH
HY_OFF = V_OFF + KV_WIDTH
GA_OFF = HY_OFF + (HY_ORDER + 1) * HY_WIDTH
GH_OFF = GA_OFF + D_MODEL
IN_WIDTH = GH_OFF + D_MODEL
D_FF = 11008
N_EXPERTS = 8
TOP_K = 2
D_FF_EXPERT = 3584
MOE_BLOCK = 256
N_DENSE = (DEPTH + 1) // 2
N_MOE = DEPTH // 2
RMS_EPS = 1e-6
F32 = jnp.float32

kernel_name = 'hybrid_dit_gqa_hyena_moe'


def rmsnorm(x, g):
    xf = x.astype(F32)
    y = xf * lax.rsqrt(jnp.mean(xf * xf, axis=-1, keepdims=True) + RMS_EPS)
    return (y * g.astype(F32)).astype(x.dtype)


def modulate(x, shift, scale):
    return x * (1 + scale) + shift


def split_heads(t, n_heads):
    return t.reshape(t.shape[:-1] + (n_heads, HEAD_DIM))


def axial_rope_tables(rows):
    row = jnp.repeat(jnp.arange(rows), GRID_W).astype(F32)
    col = jnp.tile(jnp.arange(GRID_W), rows).astype(F32)
    inv = ROPE_BASE ** (-jnp.arange(ROPE_FREQS, dtype=F32) / ROPE_FREQS)
    ang = jnp.stack([row[:, None] * inv, col[:, None] * inv], axis=1)
    return jnp.cos(ang), jnp.sin(ang)


def apply_axial_rope(x, cos, sin):
    b, n, h, _ = x.shape
    xr = x.astype(F32).reshape(b, n, h, 2, 2, ROPE_FREQS)
    x1, x2 = xr[..., 0, :], xr[..., 1, :]
    cs, sn = cos[None, :, None], sin[None, :, None]
    out = jnp.stack([x1 * cs - x2 * sn, x1 * sn + x2 * cs], axis=-2)
    return out.reshape(b, n, h, HEAD_DIM).astype(x.dtype)


def latent_window_attention(q, k, v, k_c, v_c, sink):
    b, s = q.shape[:2]
    nb = s // BLOCK
    scale = HEAD_DIM ** -0.5
    qb = q.reshape(b, nb, BLOCK, N_KV_HEADS, Q_GROUP, HEAD_DIM)
    pad = ((0, 0), (BLOCK, BLOCK), (0, 0), (0, 0))
    kp, vp = jnp.pad(k, pad), jnp.pad(v, pad)

    def band(t):
        return jnp.concatenate([t[:, j * BLOCK:j * BLOCK + s].reshape(b, nb, BLOCK, N_KV_HEADS, HEAD_DIM)
                                for j in range(3)], axis=2)

    kb, vb = band(kp), band(vp)
    s_loc = jnp.einsum('bnqhgd,bnkhd->bnhgqk', qb, kb).astype(F32) * scale
    q_pos = jnp.arange(nb)[:, None, None] * BLOCK + jnp.arange(BLOCK)[None, :, None]
    k_pos = jnp.arange(nb)[:, None, None] * BLOCK - BLOCK + jnp.arange(3 * BLOCK)[None, None, :]
    valid = (jnp.abs(q_pos - k_pos) <= WINDOW) & (k_pos >= 0) & (k_pos < s)
    s_loc = jnp.where(valid[None, :, None, None], s_loc, MASK_VALUE)
    s_ctx = jnp.einsum('bnqhgd,bchd->bnhgqc', qb, k_c).astype(F32) * scale
    s_sink = jnp.broadcast_to(sink.astype(F32).reshape(1, 1, N_KV_HEADS, Q_GROUP, 1, 1), s_ctx.shape[:-1] + (1,))
    p = jax.nn.softmax(jnp.concatenate([s_loc, s_ctx, s_sink], axis=-1), axis=-1)
    n_loc, n_ctx = 3 * BLOCK, k_c.shape[1]
    p_loc = p[..., :n_loc].astype(v.dtype)
    p_ctx = p[..., n_loc:n_loc + n_ctx].astype(v.dtype)
    o = jnp.einsum('bnhgqk,bnkhd->bnqhgd', p_loc, vb) + jnp.einsum('bnhgqc,bchd->bnqhgd', p_ctx, v_c)
    return o.reshape(b, s, ATT_WIDTH)


def context_attention(q, k, v, sink):
    b, n = q.shape[:2]
    scale = HEAD_DIM ** -0.5
    qg = q.reshape(b, n, N_KV_HEADS, Q_GROUP, HEAD_DIM)
    sc = jnp.einsum('bqhgd,bkhd->bhgqk', qg, k).astype(F32) * scale
    s_sink = jnp.broadcast_to(sink.astype(F32).reshape(1, N_KV_HEADS, Q_GROUP, 1, 1), sc.shape[:-1] + (1,))
    p = jax.nn.softmax(jnp.concatenate([sc, s_sink], axis=-1), axis=-1)
    o = jnp.einsum('bhgqk,bkhd->bqhgd', p[..., :n].astype(v.dtype), v)
    return o.reshape(b, n, ATT_WIDTH)


def short_conv(u, w, bias):
    n = u.shape[1]
    half = SHORT_CONV // 2
    up = jnp.pad(u, ((0, 0), (half, SHORT_CONV - 1 - half), (0, 0)))
    out = up[:, 0:n] * w[0]
    for j in range(1, SHORT_CONV):
        out = out + up[:, j:j + n] * w[j]
    return out + bias


def hyena_filter_spectrum(n, w1, b1, w2, b2, w3, b3, freq, w_out):
    t = jnp.linspace(0.0, 1.0, n, dtype=F32)[:, None]
    w = 2.0 * math.pi * jnp.arange(n, dtype=F32)[:, None] / n
    bands = jnp.linspace(1e-4, FILTER_BANDS - 1, FILTER_BANDS, dtype=F32)[None, :]
    z = jnp.concatenate([t, jnp.cos(bands * w), -jnp.sin(bands * w)], axis=-1)
    fr = freq.astype(F32)
    h = jnp.sin(fr * (z @ w1.astype(F32) + b1.astype(F32)))
    h = jnp.sin(fr * (h @ w2.astype(F32) + b2.astype(F32)))
    h = jnp.sin(fr * (h @ w3.astype(F32) + b3.astype(F32)))
    h = (h @ w_out.astype(F32)).reshape(n, HY_ORDER, HY_DIRS, HY_WIDTH)
    deltas = jnp.abs(jnp.linspace(MIN_DECAY, MAX_DECAY, HY_WIDTH, dtype=F32))
    h = h * jnp.exp(-t * deltas)[:, None, None, :]
    fwd, bwd = h[:, :, 0], h[:, :, 1]
    full = jnp.concatenate([fwd, jnp.zeros((1, HY_ORDER, HY_WIDTH), F32), bwd[1:][::-1]], axis=0)
    return jnp.fft.rfft(full, axis=0)


def hyena_mixer(u, conv_w, conv_b, spec, hy_bias):
    n = u.shape[1]
    uc = short_conv(u, conv_w, conv_b).astype(F32)
    v, x1, x2 = jnp.split(uc, 3, axis=-1)
    bias = hy_bias.astype(F32)
    z = v
    for o, gate in enumerate((x1, x2)):
        zf = jnp.fft.rfft(z, n=2 * n, axis=1)
        conv = jnp.fft.irfft(zf * spec[None, :, o], n=2 * n, axis=1)[:, :n]
        z = gate * (conv + z * bias[o])
    return z.astype(u.dtype)


def gated_merge(u, att, hy, w_ao, w_ho, w_o):
    ga = jax.nn.sigmoid(u[..., GA_OFF:GH_OFF])
    gh = jax.nn.sigmoid(u[..., GH_OFF:IN_WIDTH])
    return (ga * (att @ w_ao) + gh * (hy @ w_ho)) @ w_o


def swiglu(h, w1, w3, w2):
    return (jax.nn.silu(h @ w1) * (h @ w3)) @ w2


def moe_swiglu(h, w_router, w1, w3, w2):
    t, d = h.shape
    logits = (h @ w_router).astype(F32)
    top_logit, top_idx = lax.top_k(logits, TOP_K)
    gate = jax.nn.softmax(top_logit, axis=-1)
    n_assign = t * TOP_K
    flat_e = top_idx.reshape(-1)
    flat_tok = jnp.repeat(jnp.arange(t, dtype=jnp.int32), TOP_K)
    flat_gate = gate.reshape(-1)
    order = jnp.argsort(flat_e)
    e_sorted = flat_e[order]
    counts = jnp.bincount(flat_e, length=N_EXPERTS)
    padded = (counts + MOE_BLOCK - 1) // MOE_BLOCK * MOE_BLOCK
    start = jnp.cumsum(counts) - counts
    pad_end = jnp.cumsum(padded)
    pad_start = pad_end - padded
    dest = pad_start[e_sorted] + jnp.arange(n_assign) - start[e_sorted]
    n_blocks = -(-n_assign // MOE_BLOCK) + N_EXPERTS
    n_slots = n_blocks * MOE_BLOCK
    slot_tok = jnp.full((n_slots,), t, jnp.int32).at[dest].set(flat_tok[order])
    slot_gate = jnp.zeros((n_slots,), F32).at[dest].set(flat_gate[order])
    block_expert = jnp.minimum(jnp.searchsorted(pad_end, jnp.arange(n_blocks) * MOE_BLOCK, side='right'),
                               N_EXPERTS - 1)
    h_pad = jnp.concatenate([h, jnp.zeros((1, d), h.dtype)], axis=0)
    xs = h_pad[slot_tok].reshape(n_blocks, MOE_BLOCK, d)

    def expert_block(args):
        xb, e = args
        return (jax.nn.silu(xb @ w1[e]) * (xb @ w3[e])) @ w2[e]

    ys = lax.map(expert_block, (xs, block_expert)).reshape(n_slots, d)
    ys = ys * slot_gate[:, None].astype(ys.dtype)
    out = jnp.zeros((t + 1, d), h.dtype).at[slot_tok].add(ys)
    return out[:t]


def channel_mixer(h, layer, ffn_w1, ffn_w3, ffn_w2, moe_router, moe_w1, moe_w3, moe_w2):
    i = layer // 2
    if layer % 2 == 0:
        return swiglu(h, ffn_w1[i], ffn_w3[i], ffn_w2[i])
    shp = h.shape
    out = moe_swiglu(h.reshape(-1, shp[-1]), moe_router[i], moe_w1[i], moe_w3[i], moe_w2[i])
    return out.reshape(shp)


def setup_inputs(seed: int = 0) -> dict:
    key = jax.random.key(seed)
    keys = jax.random.split(key, 32)

    def nrm(i, shape, std):
        return jax.random.normal(keys[i], shape, F32) * std

    L, D = DEPTH, D_MODEL
    return {
        'x': nrm(0, (BATCH, SEQ, D), 1.0),
        'c': nrm(1, (BATCH, D), 1.0),
        'ctx': nrm(2, (BATCH, CTX_LEN, D), 1.0),
        'c_ctx': nrm(3, (D,), 1.0),
        'w_ada': nrm(4, (L, D, 6 * D), 0.5 * D ** -0.5),
        'b_ada': nrm(5, (L, 6 * D), 0.02),
        'norm_g': 1.0 + nrm(6, (L, 4, D), 0.02),
        'w_in': nrm(7, (L, D, IN_WIDTH), D ** -0.5),
        'attn_sink': nrm(8, (L, N_Q_HEADS), 0.5),
        'conv_w': nrm(9, (L, SHORT_CONV, (HY_ORDER + 1) * HY_WIDTH), SHORT_CONV ** -0.5),
        'conv_b': nrm(10, (L, (HY_ORDER + 1) * HY_WIDTH), 0.02),
        'filt_w1': nrm(11, (L, FILTER_EMB, FILTER_HIDDEN), FILTER_EMB ** -0.5),
        'filt_b1': nrm(12, (L, FILTER_HIDDEN), 0.02),
        'filt_w2': nrm(13, (L, FILTER_HIDDEN, FILTER_HIDDEN), FILTER_HIDDEN ** -0.5),
        'filt_b2': nrm(14, (L, FILTER_HIDDEN), 0.02),
        'filt_w3': nrm(15, (L, FILTER_HIDDEN, FILTER_HIDDEN), FILTER_HIDDEN ** -0.5),
        'filt_b3': nrm(16, (L, FILTER_HIDDEN), 0.02),
        'filt_freq': 1.0 + nrm(17, (L, FILTER_HIDDEN), 0.02),
        'filt_w_out': nrm(18, (L, FILTER_HIDDEN, HY_ORDER * HY_DIRS * HY_WIDTH), 0.01),
        'hyena_bias': nrm(19, (L, HY_ORDER, HY_WIDTH), 0.5),
        'w_attn_out': nrm(20, (L, ATT_WIDTH, D), ATT_WIDTH ** -0.5),
        'w_hyena_out': nrm(21, (L, HY_WIDTH, D), HY_WIDTH ** -0.5),
        'w_out': nrm(22, (L, D, D), D ** -0.5),
        'ffn_w1': nrm(23, (N_DENSE, D, D_FF), D ** -0.5),
        'ffn_w3': nrm(24, (N_DENSE, D, D_FF), D ** -0.5),
        'ffn_w2': nrm(25, (N_DENSE, D_FF, D), D_FF ** -0.5),
        'moe_router': nrm(26, (N_MOE, D, N_EXPERTS), D ** -0.5),
        'moe_w1': nrm(27, (N_MOE, N_EXPERTS, D, D_FF_EXPERT), D ** -0.5),
        'moe_w3': nrm(28, (N_MOE, N_EXPERTS, D, D_FF_EXPERT), D ** -0.5),
        'moe_w2': nrm(29, (N_MOE, N_EXPERTS, D_FF_EXPERT, D), D_FF_EXPERT ** -0.5),
    }


def reference(x, c, ctx, c_ctx, w_ada, b_ada, norm_g, w_in, attn_sink, conv_w, conv_b,
              filt_w1, filt_b1, filt_w2, filt_b2, filt_w3, filt_b3, filt_freq, filt_w_out, hyena_bias,
              w_attn_out, w_hyena_out, w_out, ffn_w1, ffn_w3, ffn_w2,
              moe_router, moe_w1, moe_w3, moe_w2):
    b, s, d = x.shape
    n_ctx = ctx.shape[1]
    rows = s // GRID_W
    rope_cos, rope_sin = axial_rope_tables(rows)
    xc = ctx
    for l in range(DEPTH):
        last = l == DEPTH - 1
        mod = jax.nn.silu(c) @ w_ada[l] + b_ada[l]
        mod_c = jax.nn.silu(c_ctx) @ w_ada[l] + b_ada[l]
        sh1, sc1, gt1, sh2, sc2, gt2 = jnp.split(mod[:, None, :], 6, axis=-1)
        csh1, csc1, cgt1, csh2, csc2, cgt2 = jnp.split(mod_c, 6)
        g_pre1, g_post1, g_pre2, g_post2 = norm_g[l]
        filt = (filt_w1[l], filt_b1[l], filt_w2[l], filt_b2[l], filt_w3[l], filt_b3[l], filt_freq[l], filt_w_out[l])

        h = modulate(rmsnorm(x, g_pre1), sh1, sc1)
        hc = modulate(rmsnorm(xc, g_pre1), csh1, csc1)
        u = h @ w_in[l]
        if last:
            kv_c = hc @ w_in[l][:, K_OFF:HY_OFF]
        else:
            u_c = hc @ w_in[l]
            kv_c = u_c[..., K_OFF:HY_OFF]
        k_c = split_heads(kv_c[..., :KV_WIDTH], N_KV_HEADS)
        v_c = split_heads(kv_c[..., KV_WIDTH:], N_KV_HEADS)

        q = apply_axial_rope(split_heads(u[..., :K_OFF], N_Q_HEADS), rope_cos, rope_sin)
        k = apply_axial_rope(split_heads(u[..., K_OFF:V_OFF], N_KV_HEADS), rope_cos, rope_sin)
        v = split_heads(u[..., V_OFF:HY_OFF], N_KV_HEADS)
        att = latent_window_attention(q, k, v, k_c, v_c, attn_sink[l])
        hy = hyena_mixer(u[..., HY_OFF:GA_OFF], conv_w[l], conv_b[l], hyena_filter_spectrum(s, *filt), hyena_bias[l])
        y = gated_merge(u, att, hy, w_attn_out[l], w_hyena_out[l], w_out[l])
        x = x + gt1 * rmsnorm(y, g_post1)
        if not last:
            q_c = split_heads(u_c[..., :K_OFF], N_Q_HEADS)
            att_c = context_attention(q_c, k_c, v_c, attn_sink[l])
            hy_c = hyena_mixer(u_c[..., HY_OFF:GA_OFF], conv_w[l], conv_b[l],
                               hyena_filter_spectrum(n_ctx, *filt), hyena_bias[l])
            y_c = gated_merge(u_c, att_c, hy_c, w_attn_out[l], w_hyena_out[l], w_out[l])
            xc = xc + cgt1 * rmsnorm(y_c, g_post1)

        h2 = modulate(rmsnorm(x, g_pre2), sh2, sc2)
        f = channel_mixer(h2, l, ffn_w1, ffn_w3, ffn_w2, moe_router, moe_w1, moe_w3, moe_w2)
        x = x + gt2 * rmsnorm(f, g_post2)
        if not last:
            h2c = modulate(rmsnorm(xc, g_pre2), csh2, csc2)
            fc = channel_mixer(h2c, l, ffn_w1, ffn_w3, ffn_w2, moe_router, moe_w1, moe_w3, moe_w2)
            xc = xc + cgt2 * rmsnorm(fc, g_post2)
    return x
```

```python
import math
from contextlib import ExitStack
import numpy as np
import ml_dtypes
import concourse.bass as bass
import concourse.mybir as mybir
from concourse.bass_utils import run_bass_kernel_spmd

F32 = mybir.dt.float32
BF16 = mybir.dt.bfloat16
I32 = mybir.dt.int32
AF = mybir.ActivationFunctionType
ALU = mybir.AluOpType
AX = mybir.AxisListType
NPBF = ml_dtypes.bfloat16

ENGS = ("pe", "act", "dve", "pool", "sp")
HD = 128
NQH, NKVH = 16, 4
ATTW, KVW = NQH * HD, NKVH * HD
RMS_EPS = 1e-6
NEG = -1e30

CFG_FULL = dict(D=4096, S=4096, C=256, HY=2048, DFF=11008, DFFE=3584, NE=8, L=2, GRID_W=64)


def derive(cfg):
    c = dict(cfg)
    c["KC"] = c["D"] // 128
    c["NL"] = c["S"] // 4
    c["NC"] = c["C"] // 4
    c["NT"] = c["NL"] + c["NC"]
    c["CHG"] = c["HY"] // 4
    c["K_OFF"] = ATTW
    c["V_OFF"] = ATTW + KVW
    c["HY_OFF"] = ATTW + 2 * KVW
    c["GA_OFF"] = c["HY_OFF"] + 3 * c["HY"]
    c["GH_OFF"] = c["GA_OFF"] + c["D"]
    c["INW"] = c["GH_OFF"] + c["D"]
    return c


def chunks(n, c=512):
    return [(o, min(c, n - o)) for o in range(0, n, c)]


class Prog:
    N_DMA_SEM = 40
    EPOCH = 30000

    def __init__(self, nc, st, nbanks=8):
        self.nc = nc
        self.st = st
        self.ops = []
        self.res = {}
        self.eng_ops = {e: [] for e in ENGS}
        self.banks = [st.enter_context(nc.psum_tensor(f"bank{i}", [128, 512], F32)) for i in range(nbanks)]
        self.bank_i = 0
        self._n = 0

    def sb(self, shape, dt, name=None):
        self._n += 1
        return self.st.enter_context(self.nc.sbuf_tensor(name or f"auto{self._n}", list(shape), dt))

    def bank(self):
        i = self.bank_i % len(self.banks)
        self.bank_i += 1
        return self.banks[i], ("bank", i)

    def op(self, eng, fn, reads=(), writes=(), dma=False):
        idx = len(self.ops)
        deps = set()
        for k in reads:
            r = self.res.setdefault(k, [None, []])
            if r[0] is not None:
                deps.add(r[0])
        for k in writes:
            r = self.res.setdefault(k, [None, []])
            if r[0] is not None:
                deps.add(r[0])
            deps.update(r[1])
        for k in reads:
            self.res[k][1].append(idx)
        for k in writes:
            self.res[k] = [idx, []]
        self.ops.append(dict(eng=eng, fn=fn, deps=deps, dma=dma, sig=False))
        self.eng_ops[eng].append(idx)
        return idx

    def dma(self, out, in_, reads=(), writes=(), q="sp"):
        return self.op(q, lambda e: e.dma_start(out=out, in_=in_), reads, writes, dma=True)

    def mm(self, out, lhsT, rhs, start, stop, reads=(), writes=()):
        return self.op("pe", lambda e: e.matmul(out, lhsT=lhsT, rhs=rhs, start=start, stop=stop), reads, writes)

    def tr(self, out, in_, ident, reads=(), writes=()):
        return self.op("pe", lambda e: e.transpose(out=out, in_=in_, identity=ident), reads, writes)

    def act(self, out, in_, func, reads=(), writes=(), bias=None, scale=None, accum_out=None):
        def f(e):
            kw = {}
            if bias is not None:
                kw["bias"] = bias
            if scale is not None:
                kw["scale"] = scale
            if accum_out is not None:
                kw["accum_out"] = accum_out
            return e.activation(out=out, in_=in_, func=func, **kw)
        return self.op("act", f, reads, writes)

    def tt(self, out, in0, in1, op, reads=(), writes=(), eng="dve"):
        return self.op(eng, lambda e: e.tensor_tensor(out=out, in0=in0, in1=in1, op=op), reads, writes)

    def ts(self, out, in0, s1, s2, op0, op1=None, reads=(), writes=(), eng="dve"):
        def f(e):
            if op1 is None:
                return e.tensor_scalar(out=out, in0=in0, scalar1=s1, scalar2=None, op0=op0)
            return e.tensor_scalar(out=out, in0=in0, scalar1=s1, scalar2=s2, op0=op0, op1=op1)
        return self.op(eng, f, reads, writes)

    def stt(self, out, in0, scalar, in1, op0, op1, reads=(), writes=()):
        return self.op("dve", lambda e: e.scalar_tensor_tensor(out=out, in0=in0, scalar=scalar, in1=in1, op0=op0, op1=op1), reads, writes)

    def cp(self, out, in_, reads=(), writes=(), eng="dve"):
        if eng == "act":
            return self.op("act", lambda e: e.copy(out=out, in_=in_), reads, writes)
        return self.op(eng, lambda e: e.tensor_copy(out=out, in_=in_), reads, writes)

    def memset(self, out, val, writes=(), eng="pool"):
        return self.op(eng, lambda e: e.memset(out, val), (), writes)

    def emit(self):
        nc, st, ops = self.nc, self.st, self.ops
        for o in ops:
            nd = set()
            for d in o["deps"]:
                po = ops[d]
                if po["eng"] == o["eng"] and not po["dma"] and o["eng"] in ("pe", "sp"):
                    continue
                nd.add(d)
                po["sig"] = True
            o["deps"] = nd
        eng_sems = {e: [] for e in ENGS}
        eng_cnt = {e: 0 for e in ENGS}
        dma_sems = [st.enter_context(nc.semaphore(f"dq{i}")) for i in range(self.N_DMA_SEM)]
        dma_cnt = [0] * self.N_DMA_SEM
        dma_last = [None] * self.N_DMA_SEM
        ndma = 0
        for i, o in enumerate(ops):
            if o["dma"]:
                s = ndma % self.N_DMA_SEM
                ndma += 1
                if dma_last[s] is not None:
                    o["deps"].add(dma_last[s])
                dma_cnt[s] += 16
                o["ticket"] = (dma_sems[s], dma_cnt[s])
                dma_last[s] = i
            elif o["sig"]:
                e = o["eng"]
                ep = eng_cnt[e] // self.EPOCH
                while len(eng_sems[e]) <= ep:
                    eng_sems[e].append(st.enter_context(nc.semaphore(f"e_{e}{len(eng_sems[e])}")))
                eng_cnt[e] += 1
                o["ticket"] = (eng_sems[e][ep], eng_cnt[e] - ep * self.EPOCH)
        for e in ENGS:
            waited = {}
            for i in self.eng_ops[e]:
                o = ops[i]
                w = {}
                for d in o["deps"]:
                    sem, val = ops[d]["ticket"]
                    key = sem.num
                    if waited.get(key, 0) >= val:
                        continue
                    if key not in w or w[key][1] < val:
                        w[key] = (sem, val)
                for key, (sem, val) in w.items():
                    waited[key] = val
                o["waits"] = list(w.values())
        block = st.enter_context(nc.Block())
        handles = {"pe": block.tensor, "act": block.scalar, "dve": block.vector, "pool": block.gpsimd, "sp": block.sync}
        for e in ENGS:
            lst = self.eng_ops[e]
            if not lst:
                continue

            def body(eng, lst=lst):
                for i in lst:
                    o = ops[i]
                    for sem, val in o["waits"]:
                        eng.wait_ge(sem, val)
                    ins = o["fn"](eng)
                    if o["dma"]:
                        ins.then_inc(o["ticket"][0], 16)
                    elif o["sig"]:
                        ins.then_inc(o["ticket"][0], 1)
            handles[e](body)


class Ring:
    def __init__(self, P, n, shape, dt, name):
        self.t = [P.sb(shape, dt, f"{name}{i}") for i in range(n)]
        self.k = [(name, i) for i in range(n)]
        self.i = 0

    def next(self):
        j = self.i % len(self.t)
        self.i += 1
        return self.t[j], self.k[j]


def dview(ap, pat, **kw):
    return ap.rearrange(pat, **kw)


class WStream:
    LA = 2

    def __init__(self, P, kmax, nst=2, nbf=3, name="w"):
        self.P = P
        self.kmax = kmax
        self.stage = Ring(P, nst, [128, kmax, 128], F32, name + "s")
        self.slots = Ring(P, nbf, [128, kmax, 128], BF16, name + "b")
        self.q = []
        self.dm = []
        self.ready = []
        self.taken = 0

    def plan(self, srcs):
        self.q.extend(srcs)

    def _dma(self):
        src = self.q[len(self.dm)]
        K = src.shape[0] // 128
        stg, sk = self.stage.next()
        self.P.dma(stg[:, 0:K, :], src.rearrange("(k p) n -> p k n", p=128), writes=[sk])
        self.dm.append((stg, sk, K))

    def _cast(self):
        i = len(self.ready)
        stg, sk, K = self.dm[i]
        slot, bk = self.slots.next()
        eng = ("pool", "act", "dve")[i % 3]
        self.P.cp(slot[:, 0:K, :], stg[:, 0:K, :], reads=[sk], writes=[bk], eng=eng)
        self.ready.append((slot, bk))

    def get(self):
        j = self.taken
        while len(self.dm) < min(len(self.q), j + 2):
            self._dma()
        while len(self.ready) < min(len(self.q), j + 2):
            self._cast()
        while len(self.dm) < min(len(self.q), j + 1 + self.LA):
            self._dma()
        self.taken += 1
        return self.ready[j]

    def load(self, src):
        self.plan([src])
        return self.get()


def rms_rstd(P, load_chunk, KC, NT, D, ones, out_rstd, out_key, sq_ring):
    ncs = chunks(NT)
    bks = [P.bank() for _ in ncs]
    for kc in range(KC):
        xa, xk = load_chunk(kc)
        sq, sqk = sq_ring.next()
        P.act(sq[:, 0:NT], xa, AF.Square, reads=[xk], writes=[sqk])
        for (n0, ns), (bt, bk) in zip(ncs, bks):
            P.mm(bt[:, 0:ns], ones[:], sq[:, n0:n0 + ns], kc == 0, kc == KC - 1, reads=[sqk, "ones"], writes=[bk])
    for (n0, ns), (bt, bk) in zip(ncs, bks):
        P.ts(out_rstd[:, n0:n0 + ns], bt[:, 0:ns], 1.0 / D, RMS_EPS, ALU.mult, ALU.add, reads=[bk], writes=[out_key])
    P.act(out_rstd[:, 0:NT], out_rstd[:, 0:NT], AF.Sqrt, reads=[out_key], writes=[out_key])
    P.op("dve", lambda e: e.reciprocal(out=out_rstd[:, 0:NT], in_=out_rstd[:, 0:NT]), reads=[out_key], writes=[out_key])


def finish(P, outs):
    P.op("sp", lambda e: None, reads=list(outs))
    P.emit()


def build_M(c):
    KC, L = c["KC"], c["L"]
    W8 = 6 * c["D"] // 8
    NJ = W8 // 128
    nc = bass.Bass("TRN2", target_bir_lowering=False)
    cT = nc.dram_tensor("cT", [128, KC, 4], F32, kind="ExternalInput").ap()
    wa = nc.dram_tensor("wa", [L, c["D"], W8], F32, kind="ExternalInput").ap()
    ba = nc.dram_tensor("ba", [128, L * NJ], F32, kind="ExternalInput").ap()
    out = nc.dram_tensor("modT", [128, L * NJ, 4], F32, kind="ExternalOutput").ap()
    with ExitStack() as st:
        P = Prog(nc, st)
        ct = P.sb([128, KC, 4], F32)
        stt_ = P.sb([128, KC, 4], F32)
        bt = P.sb([128, L * NJ], F32)
        ot = P.sb([128, L * NJ, 4], F32)
        wr = Ring(P, 3, [128, KC, 128], F32, "wst")
        P.dma(ct[:], cT, writes=["ct"])
        P.dma(bt[:], ba, writes=["bt"])
        P.act(stt_[:], ct[:], AF.Silu, reads=["ct"], writes=["st"])
        for l in range(L):
            for j in range(NJ):
                w, wk = wr.next()
                P.dma(w[:], wa[l, :, j * 128:(j + 1) * 128].rearrange("(k p) n -> p k n", p=128), writes=[wk])
                b, bk = P.bank()
                for k in range(KC):
                    P.mm(b[:, 0:4], w[:, k, :], stt_[:, k, :], k == 0, k == KC - 1, reads=[wk, "st"], writes=[bk])
                lj = l * NJ + j
                P.act(ot[:, lj, :], b[:, 0:4], AF.Identity, reads=[bk, "bt"], writes=["ot"], bias=bt[:, lj:lj + 1])
        P.dma(out, ot[:], reads=["ot"], writes=["out"])
        finish(P, ["out"])
    return nc


def build_A(c):
    KC, NT, NL, D, INW = c["KC"], c["NT"], c["NL"], c["D"], c["INW"]
    nc = bass.Bass("TRN2", target_bir_lowering=False)
    xT = nc.dram_tensor("xT", [D, NT], F32, kind="ExternalInput").ap()
    modT = nc.dram_tensor("modT", [128, 6 * KC, 2], F32, kind="ExternalInput").ap()
    g1 = nc.dram_tensor("g1", [128, KC], F32, kind="ExternalInput").ap()
    win = nc.dram_tensor("win", [D, INW], F32, kind="ExternalInput").ap()
    cosT = nc.dram_tensor("cosT", [128, NL], F32, kind="ExternalInput").ap()
    sinT = nc.dram_tensor("sinT", [128, NL], F32, kind="ExternalInput").ap()
    pm = nc.dram_tensor("pm", [128, 128], F32, kind="ExternalInput").ap()
    uT = nc.dram_tensor("uT", [INW, NT], BF16, kind="ExternalOutput").ap()
    ncs = chunks(NT)
    with ExitStack() as st:
        P = Prog(nc, st)
        ones = P.sb([128, 128], F32)
        P.memset(ones[:], 1.0, writes=["ones"])
        mt = P.sb([128, 6 * KC, 2], F32)
        gt = P.sb([128, KC], F32)
        A_ = P.sb([128, KC, 2], F32)
        cs = P.sb([128, NL], F32)
        sn = P.sb([128, NL], F32)
        pmt = P.sb([128, 128], F32)
        rstd = P.sb([128, NT], F32)
        hT = P.sb([128, KC, NT], BF16)
        P.dma(mt[:], modT, writes=["mt"])
        P.dma(gt[:], g1, writes=["gt"])
        P.dma(cs[:], cosT, writes=["cs"])
        P.dma(sn[:], sinT, writes=["sn"])
        P.dma(pmt[:], pm, writes=["pmt"])
        for col in range(2):
            P.stt(A_[:, :, col], mt[:, KC:2 * KC, col], 1.0, gt[:], ALU.add, ALU.mult, reads=["mt", "gt"], writes=["A"])
        xr = Ring(P, 3, [128, NT], F32, "xr")
        sqr = Ring(P, 2, [128, NT], F32, "sq")

        def ld(kc):
            t, k = xr.next()
            P.dma(t[:], xT[kc * 128:(kc + 1) * 128, :], writes=[k])
            return t[:], k
        rms_rstd(P, ld, KC, NT, D, ones, rstd, "rstd", sqr)
        for kc in range(KC):
            xa, xk = ld(kc)
            t, tk = sqr.next()
            P.tt(t[:], xa, rstd[:], ALU.mult, reads=[xk, "rstd"], writes=[tk])
            P.act(hT[:, kc, 0:NL], t[:, 0:NL], AF.Identity, reads=[tk, "A", "mt"], writes=[("hT", kc)],
                  bias=mt[:, kc, 0:1], scale=A_[:, kc, 0:1])
            P.act(hT[:, kc, NL:NT], t[:, NL:NT], AF.Identity, reads=[tk, "A", "mt"], writes=[("hT", kc)],
                  bias=mt[:, kc, 1:2], scale=A_[:, kc, 1:2])
        ws = WStream(P, KC)
        ws.plan([win[:, j * 128:(j + 1) * 128] for j in range(INW // 128)])
        og = Ring(P, 3, [128, NT], BF16, "og")
        xs_r = Ring(P, 2, [128, 512], F32, "xs")
        t1_r = Ring(P, 2, [128, 512], F32, "t1")
        t2_r = Ring(P, 2, [128, 512], F32, "t2")
        NJ = INW // 128
        n_rope = (ATTW + KVW) // 128
        ga_j = c["GA_OFF"] // 128
        for j in range(NJ):
            slot, sk = ws.get()
            o, ok = og.next()
            for (n0, ns) in ncs:
                b, bk = P.bank()
                for k in range(KC):
                    P.mm(b[:, 0:ns], slot[:, k, :], hT[:, k, n0:n0 + ns], k == 0, k == KC - 1, reads=[sk, ("hT", k)], writes=[bk])
                if j < n_rope:
                    l0, l1 = n0, min(n0 + ns, NL)
                    if l1 > l0:
                        w_ = l1 - l0
                        xs, xk = xs_r.next()
                        P.cp(xs[:, 0:w_], b[:, 0:w_], reads=[bk], writes=[xk], eng="act")
                        b2, b2k = P.bank()
                        P.mm(b2[:, 0:w_], pmt[:], xs[:, 0:w_], True, True, reads=["pmt", xk], writes=[b2k])
                        t1, t1k = t1_r.next()
                        t2, t2k = t2_r.next()
                        P.tt(t1[:, 0:w_], xs[:, 0:w_], cs[:, l0:l1], ALU.mult, reads=[xk, "cs"], writes=[t1k])
                        P.tt(t2[:, 0:w_], b2[:, 0:w_], sn[:, l0:l1], ALU.mult, reads=[b2k, "sn"], writes=[t2k])
                        P.tt(o[:, l0:l1], t1[:, 0:w_], t2[:, 0:w_], ALU.add, reads=[t1k, t2k], writes=[ok], eng="pool")
                    if n0 + ns > NL:
                        c0 = max(n0, NL)
                        P.cp(o[:, c0:n0 + ns], b[:, c0 - n0:ns], reads=[bk], writes=[ok], eng="act")
                elif j >= ga_j:
                    P.act(o[:, n0:n0 + ns], b[:, 0:ns], AF.Sigmoid, reads=[bk], writes=[ok])
                else:
                    if (j + n0) % 2 == 0:
                        P.cp(o[:, n0:n0 + ns], b[:, 0:ns], reads=[bk], writes=[ok], eng="act")
                    else:
                        P.cp(o[:, n0:n0 + ns], b[:, 0:ns], reads=[bk], writes=[ok], eng="dve")
            P.dma(uT[j * 128:(j + 1) * 128, :], o[:], reads=[ok], writes=["uT"], q="act")
        finish(P, ["uT"])
    return nc


def build_B1(c, with_ctx_q):
    NT, NL, NC, C = c["NT"], c["NL"], c["NC"], c["C"]
    NB = NL // 128
    NKL = NL + 256
    NK = NKL + C
    NKB = NK // 128
    CB = C // 128
    scale = HD ** -0.5
    nc = bass.Bass("TRN2", target_bir_lowering=False)
    qT = nc.dram_tensor("qT", [ATTW, NT], BF16, kind="ExternalInput").ap()
    kT = nc.dram_tensor("kT", [KVW, NK], BF16, kind="ExternalInput").ap()
    vtm = nc.dram_tensor("vtm", [128, NKB, KVW], BF16, kind="ExternalInput").ap()
    mask = nc.dram_tensor("mask", [128, NB, 384], F32, kind="ExternalInput").ap()
    sink = nc.dram_tensor("sink", [128, NQH], F32, kind="ExternalInput").ap()
    ident = nc.dram_tensor("ident", [128, 128], BF16, kind="ExternalInput").ap()
    attT = nc.dram_tensor("attT", [ATTW, NT], BF16, kind="ExternalOutput").ap()
    with ExitStack() as st:
        P = Prog(nc, st, nbanks=6)
        q_t = P.sb([128, NQH, NT], BF16)
        k_t = P.sb([128, NKVH, NK], BF16)
        v_t = P.sb([128, NKB, KVW], BF16)
        m_t = P.sb([128, NB, 384], F32)
        s_t = P.sb([128, NQH], F32)
        id_t = P.sb([128, 128], BF16)
        o_t = P.sb([128, NQH, NT], BF16)
        P.dma(q_t[:], qT.rearrange("(h p) n -> p h n", p=128), writes=["q"])
        P.dma(k_t[:], kT.rearrange("(h p) n -> p h n", p=128), writes=["k"])
        P.dma(v_t[:], vtm, writes=["v"])
        P.dma(m_t[:], mask, writes=["m"])
        P.dma(s_t[:], sink, writes=["s"])
        P.dma(id_t[:], ident, writes=["id"])
        NKEY = 384 + C
        tr_ = Ring(P, 3, [128, NKEY], F32, "t")
        er_ = Ring(P, 3, [128, NKEY], F32, "e")
        pr_ = Ring(P, 3, [128, NKEY], BF16, "p")
        sm_ = Ring(P, 6, [128, 8], F32, "sm")
        pT_ = Ring(P, 2, [128, 3 + CB, 4, 128], BF16, "pT")
        ptb = [st.enter_context(nc.psum_tensor(f"ptb{i}", [128, 8, 128], BF16)) for i in range(2)]
        ptb_i = [0]

        def attend(qcols0, qn, kblocks, use_mask, blk):
            nloc = len([kb for kb in kblocks if kb * 128 < NKL])
            nk = len(kblocks) * 128
            for g in range(NKVH):
                pT, pTk = pT_.next()
                for hh in range(4):
                    h = g * 4 + hh
                    b, bk = P.bank()
                    if nloc:
                        k0 = kblocks[0] * 128
                        P.mm(b[0:qn, 0:nloc * 128], q_t[:, h, qcols0:qcols0 + qn], k_t[:, g, k0:k0 + nloc * 128], True, True,
                             reads=["q", "k"], writes=[bk])
                    b2, b2k = P.bank()
                    P.mm(b2[0:qn, 0:C], q_t[:, h, qcols0:qcols0 + qn], k_t[:, g, NKL:NK], True, True, reads=["q", "k"], writes=[b2k])
                    t, tk = tr_.next()
                    if nloc:
                        P.stt(t[0:qn, 0:nloc * 128], b[0:qn, 0:nloc * 128], scale, m_t[0:qn, blk, use_mask[0]:use_mask[1]], ALU.mult, ALU.add,
                              reads=[bk, "m"], writes=[tk])
                    P.ts(t[0:qn, nloc * 128:nk], b2[0:qn, 0:C], scale, None, ALU.mult, reads=[b2k], writes=[tk])
                    sm, smk = sm_.next()
                    P.op("dve", lambda e, sm=sm, t=t: e.reduce_max(out=sm[0:qn, 0:1], in_=t[0:qn, 0:nk], axis=AX.X), reads=[tk], writes=[smk])
                    P.ts(sm[0:qn, 1:2], sm[0:qn, 0:1], s_t[0:qn, h:h + 1], -1.0, ALU.max, ALU.mult, reads=[smk, "s"], writes=[smk])
                    e_, ek = er_.next()
                    P.act(e_[0:qn, 0:nk], t[0:qn, 0:nk], AF.Exp, reads=[tk, smk], writes=[ek, smk], bias=sm[0:qn, 1:2], accum_out=sm[0:qn, 2:3])
                    P.act(sm[0:qn, 3:4], s_t[0:qn, h:h + 1], AF.Exp, reads=["s", smk], writes=[smk], bias=sm[0:qn, 1:2])
                    P.tt(sm[0:qn, 4:5], sm[0:qn, 2:3], sm[0:qn, 3:4], ALU.add, reads=[smk], writes=[smk])
                    P.op("dve", lambda e, sm=sm: e.reciprocal(out=sm[0:qn, 5:6], in_=sm[0:qn, 4:5]), reads=[smk], writes=[smk])
                    p_, pk = pr_.next()
                    P.ts(p_[0:qn, 0:nk], e_[0:qn, 0:nk], sm[0:qn, 5:6], None, ALU.mult, reads=[ek, smk], writes=[pk])
                    pb = ptb[ptb_i[0] % 2]
                    pbk = ("ptb", ptb_i[0] % 2)
                    ptb_i[0] += 1
                    for i in range(len(kblocks)):
                        P.tr(pb[:, i, 0:qn], p_[0:qn, i * 128:(i + 1) * 128], id_t[0:qn, 0:qn], reads=[pk, "id"], writes=[pbk])
                    P.cp(pT[:, 0:len(kblocks), hh, 0:qn], pb[:, 0:len(kblocks), 0:qn], reads=[pbk], writes=[pTk], eng=("act" if hh % 2 else "dve"))
                ob, obk = P.bank()
                for i, kb in enumerate(kblocks):
                    P.mm(ob[:, 0:4 * 128].rearrange("p (h q) -> p h q", h=4)[:, :, 0:qn] if qn < 128 else ob[:, 0:512],
                         v_t[:, kb, g * 128:(g + 1) * 128],
                         pT[:, i, :, 0:qn] if qn < 128 else pT[:, i, :, :].rearrange("p h q -> p (h q)"),
                         i == 0, i == len(kblocks) - 1, reads=["v", pTk], writes=[obk])
                src = ob[:, 0:512].rearrange("p (h q) -> p h q", h=4)[:, :, 0:qn]
                P.cp(o_t[:, g * 4:(g + 1) * 4, qcols0:qcols0 + qn], src, reads=[obk], writes=["o"], eng="act")

        ctxb = [NKL // 128 + i for i in range(CB)]
        for n in range(NB):
            attend(n * 128, 128, [n, n + 1, n + 2] + ctxb, (0, 384), n)
        if with_ctx_q:
            attend(NL, NC, ctxb, None, 0)
        else:
            P.memset(o_t[:, :, NL:NT], 0.0, writes=["o"], eng="dve")
        P.dma(attT.rearrange("(h p) n -> p h n", p=128), o_t[:], reads=["o"], writes=["out"])
        finish(P, ["out"])
    return nc


def dft_consts(n):
    N = 2 * n
    TB = n // 128
    RC = (n + 1 + 127) // 128
    IC = n // 128
    t = np.arange(n, dtype=np.float64)[:, None]
    kr = np.arange(RC * 128, dtype=np.float64)[None, :]
    ki = np.arange(IC * 128, dtype=np.float64)[None, :]
    cre = np.cos(2 * np.pi * t * kr / N) * (kr <= n)
    cim = -np.sin(2 * np.pi * t * ki / N)
    cf = np.concatenate([cre, cim], 1)
    KFC = RC + IC
    cf_h = cf.reshape(TB, 128, KFC, 128).transpose(2, 1, 0, 3)
    wk = np.where((kr == 0) | (kr == n), 1.0, 2.0) * (kr <= n)
    ire = (wk * np.cos(2 * np.pi * t * kr / N) / N).T
    iim = (-2.0 * np.sin(2 * np.pi * t * ki / N) / N).T
    ci = np.concatenate([ire, iim], 0)
    tcs = chunks(n)
    TW = tcs[0][1]
    ci_h = ci.reshape(KFC, 128, len(tcs), TW).transpose(2, 1, 0, 3)
    return np.ascontiguousarray(cf_h).astype(NPBF), np.ascontiguousarray(ci_h).astype(NPBF), RC, IC


def filt_consts(n, HY):
    t = np.linspace(0.0, 1.0, n, dtype=np.float32)[:, None]
    w = (2.0 * math.pi * np.arange(n, dtype=np.float32)[:, None] / n).astype(np.float32)
    bands = np.linspace(1e-4, 15, 16, dtype=np.float32)[None, :]
    z = np.concatenate([t, np.cos(bands * w), -np.sin(bands * w)], -1).astype(np.float32)
    mn = math.log(1e-2) / 1.5
    mx = math.log(1e-2) / 0.3
    deltas = np.abs(np.linspace(mn, mx, HY, dtype=np.float32))
    decay = np.exp(-t * deltas[None, :]).astype(np.float32)
    return np.ascontiguousarray(z.T), decay


def build_B2(c, with_ctx):
    S, C, CHG = c["S"], c["C"], c["CHG"]
    CP = min(256, CHG)
    NPASS = CHG // CP
    CPC = CP // 128
    NTOT = S + C
    seqs = [("l", S, 0)] + ([("c", C, S)] if with_ctx else [])
    nc = bass.Bass("TRN2", target_bir_lowering=False)
    uh = nc.dram_tensor("uh", [3 * CHG, NTOT], BF16, kind="ExternalInput").ap()
    cw = nc.dram_tensor("cw", [128, 3 * CHG // 128, 4], F32, kind="ExternalInput").ap()
    hb = nc.dram_tensor("hb", [128, 2, CHG // 128], F32, kind="ExternalInput").ap()
    fw1 = nc.dram_tensor("fw1", [33, 64], F32, kind="ExternalInput").ap()
    fw2 = nc.dram_tensor("fw2", [64, 64], F32, kind="ExternalInput").ap()
    fw3 = nc.dram_tensor("fw3", [64, 64], F32, kind="ExternalInput").ap()
    fbf = nc.dram_tensor("fbf", [64, 4], F32, kind="ExternalInput").ap()
    fwo = nc.dram_tensor("fwo", [64, 4, CHG], F32, kind="ExternalInput").ap()
    ident = nc.dram_tensor("ident", [128, 128], F32, kind="ExternalInput").ap()
    dr = {}
    for nm, n, _ in seqs:
        TB = n // 128
        RC = (n + 1 + 127) // 128
        IC = n // 128
        KFC = RC + IC
        tcs = chunks(n)
        dr[nm] = dict(
            zf=nc.dram_tensor("zf_" + nm, [33, n], F32, kind="ExternalInput").ap(),
            dec=nc.dram_tensor("dec_" + nm, [n, CHG], F32, kind="ExternalInput").ap(),
            cf=nc.dram_tensor("cf_" + nm, [KFC, 128, TB, 128], BF16, kind="ExternalInput").ap(),
            ci=nc.dram_tensor("ci_" + nm, [len(tcs), 128, KFC, tcs[0][1]], BF16, kind="ExternalInput").ap(),
            xs=nc.dram_tensor("xs_" + nm, [2, CP, n], F32, kind="Internal").ap(),
            TB=TB, RC=RC, IC=IC, KFC=KFC, tcs=tcs)
    hyT = nc.dram_tensor("hyT", [CHG, NTOT], F32, kind="ExternalOutput").ap()
    nmax = S
    TBm = nmax // 128
    KFCm = dr["l"]["KFC"]
    with ExitStack() as st:
        P = Prog(nc, st)
        cw_t = P.sb([128, 3 * CHG // 128, 4], F32)
        hb_t = P.sb([128, 2, CHG // 128], F32)
        w1_t = P.sb([33, 64], F32)
        w2_t = P.sb([64, 64], F32)
        w3_t = P.sb([64, 64], F32)
        bf_t = P.sb([64, 4], F32)
        wo_t = P.sb([64, 4, CHG], F32)
        id_t = P.sb([128, 128], F32)
        for t_, d_, k_ in [(cw_t, cw, "cw"), (hb_t, hb, "hb"), (w1_t, fw1, "w1"), (w2_t, fw2, "w2"), (w3_t, fw3, "w3"),
                           (bf_t, fbf, "bf"), (wo_t, fwo, "wo"), (id_t, ident, "id")]:
            P.dma(t_[:], d_, writes=[k_])
        hid3 = {nm: P.sb([64, n], F32, "hid3" + nm) for nm, n, _ in seqs}
        zT = P.sb([128, CPC, nmax], F32, "zT")
        fp_tm = P.sb([128, TBm, CP], BF16, "fp")
        fm_tm = P.sb([128, TBm, CP], BF16, "fm")
        Hs = P.sb([128, KFCm, CP], BF16, "H")
        z_tm = P.sb([128, TBm, CP], BF16, "ztm")
        CW = 512
        ub_r = Ring(P, 2, [128, CW + 2], BF16, "ub")
        co_r = Ring(P, 2, [128, CW], F32, "co")
        cf_r = Ring(P, 2, [128, TBm, 128], BF16, "cfb")
        GK = 4
        ci_r = Ring(P, 3, [128, GK, 512], BF16, "cib")
        dc_r = Ring(P, 2, [128, CP], F32, "dc")
        tm_r = Ring(P, 6, [128, 512], F32, "tm")
        zf_r = Ring(P, 2, [33, 512], F32, "zf")
        hi_r = tm_r
        ii_r = Ring(P, 2, [64, 512], I32, "ii")
        TWO_PI = float(2 * np.pi)

        for nm, n, _ in seqs:
            d = dr[nm]
            for (n0, ns) in chunks(n):
                zf, zfk = zf_r.next()
                P.dma(zf[:, 0:ns], d["zf"][:, n0:n0 + ns], writes=[zfk])
                cur, curk, kdim = zf, zfk, 33
                for li, (wt, wk) in enumerate([(w1_t, "w1"), (w2_t, "w2"), (w3_t, "w3")]):
                    b, bk = P.bank()
                    P.mm(b[0:64, 0:ns], wt[0:kdim, :], cur[0:kdim, 0:ns], True, True, reads=[wk, curk], writes=[bk])
                    pre, prk = hi_r.next()
                    P.ts(pre[0:64, 0:ns], b[0:64, 0:ns], bf_t[:, li:li + 1], bf_t[:, 3:4], ALU.add, ALU.mult, reads=[bk, "bf"], writes=[prk])
                    ii, iik = ii_r.next()
                    P.ts(ii[:, 0:ns], pre[0:64, 0:ns], 1.0 / TWO_PI, None, ALU.mult, reads=[prk], writes=[iik])
                    kf, kfk = hi_r.next()
                    P.cp(kf[0:64, 0:ns], ii[:, 0:ns], reads=[iik], writes=[kfk])
                    P.stt(pre[0:64, 0:ns], kf[0:64, 0:ns], -TWO_PI, pre[0:64, 0:ns], ALU.mult, ALU.add, reads=[kfk, prk], writes=[prk])
                    if li < 2:
                        P.act(pre[0:64, 0:ns], pre[0:64, 0:ns], AF.Sin, reads=[prk], writes=[prk])
                        cur, curk, kdim = pre, prk, 64
                    else:
                        P.act(hid3[nm][:, n0:n0 + ns], pre[0:64, 0:ns], AF.Sin, reads=[prk], writes=[("hid3", nm)])

        for ps in range(NPASS):
            ch0 = ps * CP
            for nm, n, col0 in seqs:
                d = dr[nm]
                TB, RC, IC, KFC, tcs = d["TB"], d["RC"], d["IC"], d["KFC"], d["tcs"]
                for s in range(3):
                    for cc in range(CPC):
                        row0 = s * CHG + ch0 + cc * 128
                        wi = row0 // 128
                        for (t0, tw) in chunks(n, CW):
                            ub, ubk = ub_r.next()
                            lo = max(t0 - 1, 0)
                            hi = min(t0 + tw + 1, n)
                            if t0 == 0:
                                P.memset(ub[:, 0:1], 0.0, writes=[ubk], eng="dve")
                            if t0 + tw == n:
                                P.memset(ub[:, tw + 1:tw + 2], 0.0, writes=[ubk], eng="dve")
                            P.dma(ub[:, 1 - (t0 - lo):1 - (t0 - lo) + (hi - lo)], uh[row0:row0 + 128, col0 + lo:col0 + hi], writes=[ubk])
                            if s == 0:
                                o_ap, ok = zT[:, cc, t0:t0 + tw], ("zT", cc)
                            else:
                                co, ok = co_r.next()
                                o_ap = co[:, 0:tw]
                            P.ts(o_ap, ub[:, 1:tw + 1], cw_t[:, wi, 1:2], cw_t[:, wi, 3:4], ALU.mult, ALU.add, reads=[ubk, "cw"], writes=[ok])
                            P.stt(o_ap, ub[:, 0:tw], cw_t[:, wi, 0:1], o_ap, ALU.mult, ALU.add, reads=[ubk, "cw", ok], writes=[ok])
                            P.stt(o_ap, ub[:, 2:tw + 2], cw_t[:, wi, 2:3], o_ap, ALU.mult, ALU.add, reads=[ubk, "cw", ok], writes=[ok])
                            if s > 0:
                                P.dma(d["xs"][s - 1, cc * 128:(cc + 1) * 128, t0:t0 + tw], o_ap, reads=[ok], writes=[("xs", nm, s - 1, cc)], q="act")
                for o in range(2):
                    for tb in range(TB):
                        dc, dck = dc_r.next()
                        P.dma(dc[:], d["dec"][tb * 128:(tb + 1) * 128, ch0:ch0 + CP], writes=[dck])
                        bf_, bfk = P.bank()
                        bb_, bbk = P.bank()
                        P.mm(bf_[:, 0:CP], hid3[nm][:, tb * 128:(tb + 1) * 128], wo_t[:, o * 2 + 0, ch0:ch0 + CP], True, True,
                             reads=[("hid3", nm), "wo"], writes=[bfk])
                        P.mm(bb_[:, 0:CP], hid3[nm][:, tb * 128:(tb + 1) * 128], wo_t[:, o * 2 + 1, ch0:ch0 + CP], True, True,
                             reads=[("hid3", nm), "wo"], writes=[bbk])
                        f1, f1k = tm_r.next()
                        f2, f2k = tm_r.next()
                        P.tt(f1[:, 0:CP], bf_[:, 0:CP], dc[:], ALU.mult, reads=[bfk, dck], writes=[f1k])
                        P.tt(f2[:, 0:CP], bb_[:, 0:CP], dc[:], ALU.mult, reads=[bbk, dck], writes=[f2k])
                        if tb == 0:
                            P.memset(f2[0:1, 0:CP], 0.0, writes=[f2k], eng="dve")
                        P.tt(fp_tm[:, tb, :], f1[:, 0:CP], f2[:, 0:CP], ALU.add, reads=[f1k, f2k], writes=["fp"], eng="pool")
                        P.tt(fm_tm[:, tb, :], f1[:, 0:CP], f2[:, 0:CP], ALU.subtract, reads=[f1k, f2k], writes=["fm"], eng="pool")
                    for i in range(KFC):
                        cfb, cfk = cf_r.next()
                        P.dma(cfb[:, 0:TB, :], d["cf"][i], writes=[cfk])
                        b, bk = P.bank()
                        src, srk = (fp_tm, "fp") if i < RC else (fm_tm, "fm")
                        for tb in range(TB):
                            P.mm(b[:, 0:CP], cfb[:, tb, :], src[:, tb, :], tb == 0, tb == TB - 1, reads=[cfk, srk], writes=[bk])
                        P.cp(Hs[:, i, :], b[:, 0:CP], reads=[bk], writes=[("H", i)], eng="act")
                    for tb in range(TB):
                        b, bk = P.bank()
                        for cc in range(CPC):
                            P.tr(b[:, cc * 128:(cc + 1) * 128], zT[:, cc, tb * 128:(tb + 1) * 128], id_t[:], reads=[("zT", cc), "id"], writes=[bk])
                        P.cp(z_tm[:, tb, :], b[:, 0:CP], reads=[bk], writes=["ztm"], eng=("act" if tb % 2 else "dve"))
                    for i in range(RC):
                        cfb, cfk = cf_r.next()
                        P.dma(cfb[:, 0:TB, :], d["cf"][i], writes=[cfk])
                        br, brk = P.bank()
                        for tb in range(TB):
                            P.mm(br[:, 0:CP], cfb[:, tb, :], z_tm[:, tb, :], tb == 0, tb == TB - 1, reads=[cfk, "ztm"], writes=[brk])
                        if i < IC:
                            cfb2, cfk2 = cf_r.next()
                            P.dma(cfb2[:, 0:TB, :], d["cf"][RC + i], writes=[cfk2])
                            bi, bik = P.bank()
                            for tb in range(TB):
                                P.mm(bi[:, 0:CP], cfb2[:, tb, :], z_tm[:, tb, :], tb == 0, tb == TB - 1, reads=[cfk2, "ztm"], writes=[bik])
                            a1, a1k = tm_r.next()
                            a2, a2k = tm_r.next()
                            a3, a3k = tm_r.next()
                            a4, a4k = tm_r.next()
                            P.tt(a1[:, 0:CP], br[:, 0:CP], Hs[:, i, :], ALU.mult, reads=[brk, ("H", i)], writes=[a1k])
                            P.tt(a2[:, 0:CP], bi[:, 0:CP], Hs[:, RC + i, :], ALU.mult, reads=[bik, ("H", RC + i)], writes=[a2k])
                            P.tt(a3[:, 0:CP], br[:, 0:CP], Hs[:, RC + i, :], ALU.mult, reads=[brk, ("H", RC + i)], writes=[a3k])
                            P.tt(a4[:, 0:CP], bi[:, 0:CP], Hs[:, i, :], ALU.mult, reads=[bik, ("H", i)], writes=[a4k])
                            P.tt(Hs[:, i, :], a1[:, 0:CP], a2[:, 0:CP], ALU.subtract, reads=[a1k, a2k], writes=[("H", i)], eng="pool")
                            P.tt(Hs[:, RC + i, :], a3[:, 0:CP], a4[:, 0:CP], ALU.add, reads=[a3k, a4k], writes=[("H", RC + i)], eng="pool")
                        else:
                            a1, a1k = tm_r.next()
                            P.tt(a1[:, 0:CP], br[:, 0:CP], Hs[:, i, :], ALU.mult, reads=[brk, ("H", i)], writes=[a1k])
                            P.cp(Hs[:, i, :], a1[:, 0:CP], reads=[a1k], writes=[("H", i)], eng="pool")
                    for tn, (t0, tw) in enumerate(tcs):
                        obs = [P.bank() for _ in range(CPC)]
                        for g0 in range(0, KFC, GK):
                            gn = min(GK, KFC - g0)
                            cib, cik = ci_r.next()
                            P.dma(cib[:, 0:gn, 0:tw], d["ci"][tn, :, g0:g0 + gn, :], writes=[cik])
                            for gi in range(gn):
                                i = g0 + gi
                                for cc in range(CPC):
                                    P.mm(obs[cc][0][:, 0:tw], Hs[:, i, cc * 128:(cc + 1) * 128], cib[:, gi, 0:tw], i == 0, i == KFC - 1,
                                         reads=[("H", i), cik], writes=[obs[cc][1]])
                        for cc in range(CPC):
                            xg, xgk = tm_r.next()
                            P.dma(xg[:, 0:tw], d["xs"][o, cc * 128:(cc + 1) * 128, t0:t0 + tw], reads=[("xs", nm, o, cc)], writes=[xgk])
                            tmp, tmk = tm_r.next()
                            gch = (ch0 // 128) + cc
                            P.stt(tmp[:, 0:tw], zT[:, cc, t0:t0 + tw], hb_t[:, o, gch:gch + 1], obs[cc][0][:, 0:tw], ALU.mult, ALU.add,
                                  reads=[("zT", cc), "hb", obs[cc][1]], writes=[tmk])
                            P.tt(zT[:, cc, t0:t0 + tw], tmp[:, 0:tw], xg[:, 0:tw], ALU.mult, reads=[tmk, xgk], writes=[("zT", cc)])
                for cc in range(CPC):
                    P.dma(hyT[ch0 + cc * 128:ch0 + (cc + 1) * 128, col0:col0 + n], zT[:, cc, 0:n], reads=[("zT", cc)], writes=["out"], q="act")
        if not with_ctx:
            zz = P.sb([128, C], F32, "zz")
            P.memset(zz[:], 0.0, writes=["zz"])
            for r0 in range(0, CHG, 128):
                P.dma(hyT[r0:r0 + 128, S:S + C], zz[:], reads=["zz"], writes=["out"])
        finish(P, ["out"])
    return nc


def build_C(c, moe, NT):
    KC, D, NL = c["KC"], c["D"], c["NL"]
    NLc = min(NL, NT)
    DFF = c["NE"] * c["DFFE"] if moe else c["DFF"]
    NE = c["NE"]
    FC = DFF // 128
    AK = ATTW // 128
    HK = c["HY"] // 128
    ncs = chunks(NT)
    nc = bass.Bass("TRN2", target_bir_lowering=False)
    attT = nc.dram_tensor("attT", [ATTW, NT], BF16, kind="ExternalInput").ap()
    hyT = nc.dram_tensor("hyT", [c["HY"], NT], F32, kind="ExternalInput").ap()
    gT = nc.dram_tensor("gT", [2 * D, NT], BF16, kind="ExternalInput").ap()
    xT = nc.dram_tensor("xT", [D, NT], F32, kind="ExternalInput").ap()
    modT = nc.dram_tensor("modT", [128, 6 * KC, 2], F32, kind="ExternalInput").ap()
    gn = nc.dram_tensor("gn", [128, 3, KC], F32, kind="ExternalInput").ap()
    wao = nc.dram_tensor("wao", [ATTW, D], F32, kind="ExternalInput").ap()
    who = nc.dram_tensor("who", [c["HY"], D], F32, kind="ExternalInput").ap()
    wo = nc.dram_tensor("wo", [D, D], F32, kind="ExternalInput").ap()
    w1 = nc.dram_tensor("w1", [D, DFF], F32, kind="ExternalInput").ap() if not moe else nc.dram_tensor("w1", [NE, D, c["DFFE"]], F32, kind="ExternalInput").ap()
    w3 = nc.dram_tensor("w3", [D, DFF], F32, kind="ExternalInput").ap() if not moe else nc.dram_tensor("w3", [NE, D, c["DFFE"]], F32, kind="ExternalInput").ap()
    w2 = nc.dram_tensor("w2", [DFF, D], F32, kind="ExternalInput").ap()
    if moe:
        wr = nc.dram_tensor("wr", [128, KC, NE], F32, kind="ExternalInput").ap()
        identf = nc.dram_tensor("identf", [128, 128], F32, kind="ExternalInput").ap()
        esel = nc.dram_tensor("esel", [NE, NE, 128], F32, kind="ExternalInput").ap()
    xo = nc.dram_tensor("xo", [D, NT], F32, kind="ExternalOutput").ap()
    mS = nc.dram_tensor("mS", [D, NT], BF16, kind="Internal").ap()
    yS = nc.dram_tensor("yS", [D, NT], F32, kind="Internal").ap()
    x1S = nc.dram_tensor("x1S", [D, NT], F32, kind="Internal").ap()
    fS = nc.dram_tensor("fS", [D, NT], F32, kind="Internal").ap()
    if moe:
        EC = c["DFFE"] // 128
        half = (EC + 1) // 2
        blocks = []
        for e in range(NE):
            blocks.append((e, e * EC, half))
            if EC - half > 0:
                blocks.append((e, e * EC + half, EC - half))
    else:
        nb = (FC + 17) // 18
        base = FC // nb
        rem = FC % nb
        blocks, s0 = [], 0
        for i in range(nb):
            ln = base + (1 if i < rem else 0)
            blocks.append((None, s0, ln))
            s0 += ln
    FB = max(b[2] for b in blocks)
    with ExitStack() as st:
        P = Prog(nc, st)
        ones = P.sb([128, 128], F32)
        P.memset(ones[:], 1.0, writes=["ones"])
        mt = P.sb([128, 6 * KC, 2], F32)
        gt = P.sb([128, 3, KC], F32)
        G1 = P.sb([128, KC, 2], F32)
        A2 = P.sb([128, KC, 2], F32)
        G2 = P.sb([128, KC, 2], F32)
        P.dma(mt[:], modT, writes=["mt"])
        P.dma(gt[:], gn, writes=["gt"])
        for col in range(2):
            P.tt(G1[:, :, col], mt[:, 2 * KC:3 * KC, col], gt[:, 0, :], ALU.mult, reads=["mt", "gt"], writes=["G1"])
            P.stt(A2[:, :, col], mt[:, 4 * KC:5 * KC, col], 1.0, gt[:, 1, :], ALU.add, ALU.mult, reads=["mt", "gt"], writes=["A2"])
            P.tt(G2[:, :, col], mt[:, 5 * KC:6 * KC, col], gt[:, 2, :], ALU.mult, reads=["mt", "gt"], writes=["G2"])
        big1 = P.sb([128, max(KC, AK + HK), NT], BF16, "big1")
        big2 = P.sb([128, FB, NT], BF16, "big2")
        rstd = P.sb([128, NT], F32, "rstd")
        ws = WStream(P, max(KC, AK, HK, FB))
        f32r = Ring(P, 7, [128, NT], F32, "f")
        b16r = Ring(P, 3, [128, NT], BF16, "g")
        P.dma(big1[:, 0:AK, :], attT.rearrange("(k p) n -> p k n", p=128), writes=[("b1", k) for k in range(AK)])
        for k in range(HK):
            t, tk = f32r.next()
            P.dma(t[:], hyT[k * 128:(k + 1) * 128, :], writes=[tk])
            P.cp(big1[:, AK + k, :], t[:], reads=[tk], writes=[("b1", AK + k)], eng="pool")
        for i in range(KC):
            ws.plan([wao[:, i * 128:(i + 1) * 128], who[:, i * 128:(i + 1) * 128]])
        ws.plan([wo[:, i * 128:(i + 1) * 128] for i in range(KC)])
        for i in range(KC):
            sa, sak = ws.get()
            sh, shk = ws.get()
            ga, gak = b16r.next()
            gh, ghk = b16r.next()
            P.dma(ga[:], gT[i * 128:(i + 1) * 128, :], writes=[gak])
            P.dma(gh[:], gT[D + i * 128:D + (i + 1) * 128, :], writes=[ghk])
            mo, mok = b16r.next()
            for (n0, ns) in ncs:
                ba, bak = P.bank()
                bh, bhk = P.bank()
                for k in range(AK):
                    P.mm(ba[:, 0:ns], sa[:, k, :], big1[:, k, n0:n0 + ns], k == 0, k == AK - 1, reads=[sak, ("b1", k)], writes=[bak])
                for k in range(HK):
                    P.mm(bh[:, 0:ns], sh[:, k, :], big1[:, AK + k, n0:n0 + ns], k == 0, k == HK - 1, reads=[shk, ("b1", AK + k)], writes=[bhk])
                t1, t1k = f32r.next()
                t2, t2k = f32r.next()
                P.tt(t1[:, 0:ns], ba[:, 0:ns], ga[:, n0:n0 + ns], ALU.mult, reads=[bak, gak], writes=[t1k])
                P.tt(t2[:, 0:ns], bh[:, 0:ns], gh[:, n0:n0 + ns], ALU.mult, reads=[bhk, ghk], writes=[t2k])
                P.tt(mo[:, n0:n0 + ns], t1[:, 0:ns], t2[:, 0:ns], ALU.add, reads=[t1k, t2k], writes=[mok], eng="pool")
            P.dma(mS[i * 128:(i + 1) * 128, :], mo[:], reads=[mok], writes=[("mS", i)], q="act")
        for k in range(KC):
            P.dma(big1[:, k, :], mS[k * 128:(k + 1) * 128, :], reads=[("mS", k)], writes=[("b1", k)])
        for i in range(KC):
            so, sok = ws.get()
            yo, yok = f32r.next()
            for (n0, ns) in ncs:
                b, bk = P.bank()
                for k in range(KC):
                    P.mm(b[:, 0:ns], so[:, k, :], big1[:, k, n0:n0 + ns], k == 0, k == KC - 1, reads=[sok, ("b1", k)], writes=[bk])
                P.cp(yo[:, n0:n0 + ns], b[:, 0:ns], reads=[bk], writes=[yok], eng="act")
            P.dma(yS[i * 128:(i + 1) * 128, :], yo[:], reads=[yok], writes=[("yS", i)], q="act")

        def ld_from(dr_, key):
            def f(kc):
                t, tk = f32r.next()
                P.dma(t[:], dr_[kc * 128:(kc + 1) * 128, :], reads=([(key, kc)] if key else []), writes=[tk])
                return t[:], tk
            return f

        def residual(src, skey, xin, xkey, G, dst, dkey):
            for kc in range(KC):
                ya, yk = ld_from(src, skey)(kc)
                xa, xk = ld_from(xin, xkey)(kc)
                t, tk = f32r.next()
                P.tt(t[:], ya, rstd[:], ALU.mult, reads=[yk, "rstd"], writes=[tk])
                o, ok = f32r.next()
                P.stt(o[:, 0:NLc], t[:, 0:NLc], G[:, kc, 0:1], xa[:, 0:NLc], ALU.mult, ALU.add, reads=[tk, xk, "G1", "G2"], writes=[ok])
                if NT > NLc:
                    P.stt(o[:, NLc:NT], t[:, NLc:NT], G[:, kc, 1:2], xa[:, NLc:NT], ALU.mult, ALU.add, reads=[tk, xk, "G1", "G2"], writes=[ok])
                P.dma(dst[kc * 128:(kc + 1) * 128, :], o[:], reads=[ok], writes=[(dkey, kc)], q="act")

        sqr = f32r
        rms_rstd(P, ld_from(yS, "yS"), KC, NT, D, ones, rstd, "rstd", sqr)
        residual(yS, "yS", xT, None, G1, x1S, "x1S")
        rms_rstd(P, ld_from(x1S, "x1S"), KC, NT, D, ones, rstd, "rstd", sqr)
        if moe:
            wr_t = P.sb([128, KC, NE], F32, "wr_sb")
            idf = P.sb([128, 128], F32, "idf")
            es_t = P.sb([NE, NE, 128], F32, "es")
            P.dma(wr_t[:], wr, writes=["wr"])
            P.dma(idf[:], identf, writes=["idf"])
            P.dma(es_t[:], esel, writes=["es"])
            lgb = [P.bank() for _ in ncs]
        for kc in range(KC):
            xa, xk = ld_from(x1S, "x1S")(kc)
            t, tk = f32r.next()
            P.tt(t[:], xa, rstd[:], ALU.mult, reads=[xk, "rstd"], writes=[tk])
            h32, hk = f32r.next()
            P.act(h32[:, 0:NLc], t[:, 0:NLc], AF.Identity, reads=[tk, "A2", "mt"], writes=[hk], bias=mt[:, 3 * KC + kc, 0:1], scale=A2[:, kc, 0:1])
            if NT > NLc:
                P.act(h32[:, NLc:NT], t[:, NLc:NT], AF.Identity, reads=[tk, "A2", "mt"], writes=[hk], bias=mt[:, 3 * KC + kc, 1:2], scale=A2[:, kc, 1:2])
            P.cp(big1[:, kc, :], h32[:], reads=[hk], writes=[("b1", kc)], eng="pool")
            if moe:
                for (n0, ns), (bt, bk) in zip(ncs, lgb):
                    P.mm(bt[0:NE, 0:ns], wr_t[:, kc, :], h32[:, n0:n0 + ns], kc == 0, kc == KC - 1, reads=["wr", hk], writes=[bk])
        if moe:
            NTT = (NT + 127) // 128
            lgT = P.sb([NE, NT], F32, "lgT")
            for (n0, ns), (bt, bk) in zip(ncs, lgb):
                P.cp(lgT[:, n0:n0 + ns], bt[0:NE, 0:ns], reads=[bk], writes=["lgT"])
            lg = P.sb([128, NTT, NE], F32, "lg")
            wk_ = P.sb([128, NTT, NE], F32, "wk")
            sm = P.sb([128, NTT, 4], F32, "smx")
            gwT = P.sb([NE, NT], F32, "gwT")
            P.memset(lg[:], 0.0, writes=["lg"], eng="dve")
            for tt_ in range(NTT):
                rows = min(128, NT - tt_ * 128)
                b, bk = P.bank()
                P.tr(b[0:rows, 0:NE], lgT[:, tt_ * 128:tt_ * 128 + rows], idf[0:NE, 0:NE], reads=["lgT", "idf"], writes=[bk])
                P.cp(lg[0:rows, tt_, :], b[0:rows, 0:NE], reads=[bk], writes=["lg"])
            for tt_ in range(NTT):
                L_ = lg[:, tt_, :]
                W_ = wk_[:, tt_, :]
                P.op("dve", lambda e, L_=L_, tt_=tt_: e.reduce_max(out=sm[:, tt_, 0:1], in_=L_, axis=AX.X), reads=["lg"], writes=["smx"])
                P.ts(W_, L_, sm[:, tt_, 0:1], NEG, ALU.is_equal, ALU.mult, reads=["lg", "smx"], writes=["wk"])
                P.tt(W_, W_, L_, ALU.add, reads=["wk", "lg"], writes=["wk"])
                P.op("dve", lambda e, W_=W_, tt_=tt_: e.reduce_max(out=sm[:, tt_, 1:2], in_=W_, axis=AX.X), reads=["wk"], writes=["smx"])
                P.ts(sm[:, tt_, 2:3], sm[:, tt_, 0:1], -1.0, None, ALU.mult, reads=["smx"], writes=["smx"])
                P.act(W_, L_, AF.Exp, reads=["lg", "smx", "wk"], writes=["wk"], bias=sm[:, tt_, 2:3])
                P.act(sm[:, tt_, 3:4], sm[:, tt_, 1:2], AF.Exp, reads=["smx"], writes=["smx"], bias=sm[:, tt_, 2:3])
                P.ts(sm[:, tt_, 3:4], sm[:, tt_, 3:4], 1.0, None, ALU.add, reads=["smx"], writes=["smx"])
                P.op("dve", lambda e, tt_=tt_: e.reciprocal(out=sm[:, tt_, 3:4], in_=sm[:, tt_, 3:4]), reads=["smx"], writes=["smx"])
                P.ts(L_, L_, sm[:, tt_, 1:2], None, ALU.is_ge, reads=["lg", "smx"], writes=["lg"])
                P.tt(W_, W_, L_, ALU.mult, reads=["wk", "lg"], writes=["wk"])
                P.ts(W_, W_, sm[:, tt_, 3:4], None, ALU.mult, reads=["wk", "smx"], writes=["wk"])
                rows = min(128, NT - tt_ * 128)
                b, bk = P.bank()
                P.tr(b[0:NE, 0:rows], wk_[0:rows, tt_, :], idf[0:rows, 0:rows], reads=["wk", "idf"], writes=[bk])
                P.cp(gwT[:, tt_ * 128:tt_ * 128 + rows], b[0:NE, 0:rows], reads=[bk], writes=["gwT"])
            gbc = P.sb([128, NT], F32, "gbc")
        for bi, (e_, c0, ln) in enumerate(blocks):
            for jj in range(ln):
                j = c0 + jj
                if moe:
                    jl = j - e_ * (c["DFFE"] // 128)
                    ws.plan([w1[e_, :, jl * 128:(jl + 1) * 128], w3[e_, :, jl * 128:(jl + 1) * 128]])
                else:
                    ws.plan([w1[:, j * 128:(j + 1) * 128], w3[:, j * 128:(j + 1) * 128]])
            ws.plan([w2[c0 * 128:(c0 + ln) * 128, i * 128:(i + 1) * 128] for i in range(KC)])
        for bi, (e_, c0, ln) in enumerate(blocks):
            if moe and (bi == 0 or blocks[bi - 1][0] != e_):
                for (n0, ns) in ncs:
                    b, bk = P.bank()
                    P.mm(b[:, 0:ns], es_t[:, e_, :], gwT[:, n0:n0 + ns], True, True, reads=["es", "gwT"], writes=[bk])
                    P.cp(gbc[:, n0:n0 + ns], b[:, 0:ns], reads=[bk], writes=["gbc"], eng="act")
            for jj in range(ln):
                j = c0 + jj
                s1, s1k = ws.get()
                s3, s3k = ws.get()
                for (n0, ns) in ncs:
                    bg, bgk = P.bank()
                    bu, buk = P.bank()
                    for k in range(KC):
                        P.mm(bg[:, 0:ns], s1[:, k, :], big1[:, k, n0:n0 + ns], k == 0, k == KC - 1, reads=[s1k, ("b1", k)], writes=[bgk])
                    for k in range(KC):
                        P.mm(bu[:, 0:ns], s3[:, k, :], big1[:, k, n0:n0 + ns], k == 0, k == KC - 1, reads=[s3k, ("b1", k)], writes=[buk])
                    sg, sgk = f32r.next()
                    P.act(sg[:, 0:ns], bg[:, 0:ns], AF.Silu, reads=[bgk], writes=[sgk])
                    if moe:
                        t2, t2k = f32r.next()
                        P.tt(t2[:, 0:ns], bu[:, 0:ns], sg[:, 0:ns], ALU.mult, reads=[buk, sgk], writes=[t2k])
                        P.tt(big2[:, jj, n0:n0 + ns], t2[:, 0:ns], gbc[:, n0:n0 + ns], ALU.mult, reads=[t2k, "gbc"], writes=[("b2", jj)], eng="pool")
                    else:
                        P.tt(big2[:, jj, n0:n0 + ns], bu[:, 0:ns], sg[:, 0:ns], ALU.mult, reads=[buk, sgk], writes=[("b2", jj)])
            for i in range(KC):
                s2, s2k = ws.get()
                fo, fok = f32r.next()
                if bi > 0:
                    P.dma(fo[:], fS[i * 128:(i + 1) * 128, :], reads=[("fS", i)], writes=[fok])
                for (n0, ns) in ncs:
                    b, bk = P.bank()
                    for k in range(ln):
                        P.mm(b[:, 0:ns], s2[:, k, :], big2[:, k, n0:n0 + ns], k == 0, k == ln - 1, reads=[s2k, ("b2", k)], writes=[bk])
                    if bi > 0:
                        P.tt(fo[:, n0:n0 + ns], b[:, 0:ns], fo[:, n0:n0 + ns], ALU.add, reads=[bk, fok], writes=[fok])
                    else:
                        P.cp(fo[:, n0:n0 + ns], b[:, 0:ns], reads=[bk], writes=[fok], eng="act")
                P.dma(fS[i * 128:(i + 1) * 128, :], fo[:], reads=[fok], writes=[("fS", i)], q="act")
        rms_rstd(P, ld_from(fS, "fS"), KC, NT, D, ones, rstd, "rstd", sqr)
        residual(fS, "fS", x1S, "x1S", G2, xo, "xo")
        finish(P, [("xo", k) for k in range(KC)])
    return nc


def fm(a, p=128):
    a = np.asarray(a)
    return np.ascontiguousarray(a.reshape((a.shape[0] // p, p) + a.shape[1:]).swapaxes(0, 1))


def run(nc, maps):
    res = run_bass_kernel_spmd(nc, maps, core_ids=list(range(8)))
    return res.results


def rope_tables(c):
    S, GW = c["S"], c["GRID_W"]
    row = np.repeat(np.arange(S // GW), GW).astype(np.float32)
    col = np.tile(np.arange(GW), S // GW).astype(np.float32)
    inv = (10000.0 ** (-np.arange(32, dtype=np.float32) / 32)).astype(np.float32)
    ar = row[:, None] * inv
    ac = col[:, None] * inv
    cosT = np.concatenate([np.cos(ar), np.cos(ar), np.cos(ac), np.cos(ac)], 1).T
    sinT = np.concatenate([-np.sin(ar), np.sin(ar), -np.sin(ac), np.sin(ac)], 1).T
    pm = np.zeros((128, 128), np.float32)
    for m in range(128):
        k = m + 32 if (m % 64) < 32 else m - 32
        pm[k, m] = 1.0
    return np.ascontiguousarray(cosT, np.float32), np.ascontiguousarray(sinT, np.float32), pm


def kernel_impl(inp, cfg, dbg=None, stop_after=None):
    c = derive(cfg)
    D, S, C, KC, NL, NC, NT, L, HY, CHG, INW = c["D"], c["S"], c["C"], c["KC"], c["NL"], c["NC"], c["NT"], c["L"], c["HY"], c["CHG"], c["INW"]
    f32 = np.float32
    g = {k: np.asarray(v) for k, v in inp.items()}
    cores = [(r // 4, r % 4) for r in range(8)]
    W8 = 6 * D // 8
    NJ = W8 // 128
    cstack = np.stack([g["c"][0], g["c"][1], g["c_ctx"], g["c_ctx"]], 1).astype(f32)
    cT = fm(cstack)
    maps = []
    for r in range(8):
        wa = np.ascontiguousarray(g["w_ada"][:, :, r * W8:(r + 1) * W8])
        ba = np.ascontiguousarray(g["b_ada"][:, r * W8:(r + 1) * W8].reshape(L, NJ, 128).transpose(2, 0, 1).reshape(128, L * NJ))
        maps.append(dict(cT=cT, wa=wa, ba=ba))
    resM = run(build_M(c), maps)
    mod = np.zeros((L, 6 * D, 4), f32)
    for r in range(8):
        m = resM[r]["modT"].reshape(128, L, NJ, 4)
        mod[:, r * W8:(r + 1) * W8, :] = m.transpose(1, 2, 0, 3).reshape(L, W8, 4)
    if dbg is not None:
        dbg["mod"] = mod
    if stop_after == "M":
        return None

    def modT_for(l, b):
        m = mod[l][:, [b, 2]]
        return fm(m)

    cosT, sinT, pm = rope_tables(c)
    ident_bf = np.eye(128, dtype=f32).astype(NPBF)
    ident_f = np.eye(128, dtype=f32)
    xT = []
    for (b, q) in cores:
        xl = g["x"][b, q * NL:(q + 1) * NL, :]
        xc = g["ctx"][b, q * NC:(q + 1) * NC, :]
        xT.append(np.ascontiguousarray(np.concatenate([xl, xc], 0).T))
    ncA = build_A(c)
    dcon = {"l": dft_consts(S), "c": dft_consts(C)}
    fcon = {"l": filt_consts(S, HY), "c": filt_consts(C, HY)}
    for l in range(L):
        last = l == L - 1
        maps = []
        for r, (b, q) in enumerate(cores):
            maps.append(dict(xT=xT[r], modT=modT_for(l, b), g1=fm(g["norm_g"][l, 0]), win=g["w_in"][l],
                             cosT=np.ascontiguousarray(cosT[:, q * NL:(q + 1) * NL]),
                             sinT=np.ascontiguousarray(sinT[:, q * NL:(q + 1) * NL]), pm=pm))
        resA = run(ncA, maps)
        uT = [resA[r]["uT"] for r in range(8)]
        if dbg is not None:
            dbg[("uT", l)] = uT
        if stop_after == ("A", l):
            return None
        NB = NL // 128
        maps = []
        for r, (b, q) in enumerate(cores):
            grp = [4 * b + j for j in range(4)]
            kfull = np.concatenate([uT[j][c["K_OFF"]:c["V_OFF"], 0:NL] for j in grp], 1)
            vfull = np.concatenate([uT[j][c["V_OFF"]:c["HY_OFF"], 0:NL] for j in grp], 1)
            kctx = np.concatenate([uT[j][c["K_OFF"]:c["V_OFF"], NL:NT] for j in grp], 1)
            vctx = np.concatenate([uT[j][c["V_OFF"]:c["HY_OFF"], NL:NT] for j in grp], 1)
            zpad = np.zeros((KVW, 128), NPBF)
            kp = np.concatenate([zpad, kfull, zpad], 1)
            vp = np.concatenate([zpad, vfull, zpad], 1)
            kext = np.concatenate([kp[:, q * NL:q * NL + NL + 256], kctx], 1)
            vext = np.concatenate([vp[:, q * NL:q * NL + NL + 256], vctx], 1)
            NK = kext.shape[1]
            vtm = np.ascontiguousarray(vext.T.reshape(NK // 128, 128, KVW).transpose(1, 0, 2))
            mask = np.zeros((128, NB, 384), f32)
            i_ = np.arange(128)[:, None]
            j_ = np.arange(384)[None, :]
            for n in range(NB):
                qpos = q * NL + n * 128 + i_
                kpos = q * NL + n * 128 - 128 + j_
                valid = (np.abs(qpos - kpos) <= 128) & (kpos >= 0) & (kpos < S)
                mask[:, n, :] = np.where(valid, 0.0, NEG)
            sink = np.ascontiguousarray(np.broadcast_to(g["attn_sink"][l][None, :], (128, NQH))).astype(f32)
            maps.append(dict(qT=np.ascontiguousarray(uT[r][0:ATTW]), kT=np.ascontiguousarray(kext), vtm=vtm, mask=mask, sink=sink, ident=ident_bf))
        resB1 = run(build_B1(c, not last), maps)
        if dbg is not None:
            dbg[("att", l)] = [resB1[r]["attT"] for r in range(8)]
        if stop_after == ("B1", l):
            return None
        maps = []
        for r, (b, gq) in enumerate(cores):
            grp = [4 * b + j for j in range(4)]
            rows = np.concatenate([np.arange(c["HY_OFF"] + s * HY + gq * CHG, c["HY_OFF"] + s * HY + (gq + 1) * CHG) for s in range(3)])
            ulat = np.concatenate([uT[j][rows, 0:NL] for j in grp], 1)
            uctx = np.concatenate([uT[j][rows, NL:NT] for j in grp], 1)
            uh = np.ascontiguousarray(np.concatenate([ulat, uctx], 1))
            crow = rows - c["HY_OFF"]
            cwv = np.stack([g["conv_w"][l, 0, crow], g["conv_w"][l, 1, crow], g["conv_w"][l, 2, crow], g["conv_b"][l, crow]], 1).astype(f32)
            hbv = fm(np.ascontiguousarray(g["hyena_bias"][l][:, gq * CHG:(gq + 1) * CHG].T)).transpose(0, 2, 1)
            fbf = np.stack([g["filt_b1"][l], g["filt_b2"][l], g["filt_b3"][l], g["filt_freq"][l]], 1).astype(f32)
            fwo = np.ascontiguousarray(g["filt_w_out"][l].reshape(64, 4, HY)[:, :, gq * CHG:(gq + 1) * CHG])
            m = dict(uh=uh, cw=fm(cwv), hb=np.ascontiguousarray(hbv), fw1=g["filt_w1"][l], fw2=g["filt_w2"][l], fw3=g["filt_w3"][l],
                     fbf=fbf, fwo=fwo, ident=ident_f)
            for nm in (["l"] if last else ["l", "c"]):
                m["zf_" + nm] = fcon[nm][0]
                m["dec_" + nm] = np.ascontiguousarray(fcon[nm][1][:, gq * CHG:(gq + 1) * CHG])
                m["cf_" + nm] = dcon[nm][0]
                m["ci_" + nm] = dcon[nm][1]
            maps.append(m)
        resB2 = run(build_B2(c, not last), maps)
        if dbg is not None:
            dbg[("hy", l)] = [resB2[r]["hyT"] for r in range(8)]
        if stop_after == ("B2", l):
            return None
        NTc = NL if last else NT
        maps = []
        for r, (b, q) in enumerate(cores):
            grp = [4 * b + j for j in range(4)]
            hy_l = np.concatenate([resB2[j]["hyT"][:, q * NL:(q + 1) * NL] for j in grp], 0)
            hy_c = np.concatenate([resB2[j]["hyT"][:, S + q * NC:S + (q + 1) * NC] for j in grp], 0)
            hyT = np.concatenate([hy_l, hy_c], 1)[:, 0:NTc]
            m = dict(attT=np.ascontiguousarray(resB1[r]["attT"][:, 0:NTc]), hyT=np.ascontiguousarray(hyT),
                     gT=np.ascontiguousarray(uT[r][c["GA_OFF"]:INW, 0:NTc]), xT=np.ascontiguousarray(xT[r][:, 0:NTc]),
                     modT=modT_for(l, b), gn=np.ascontiguousarray(fm(g["norm_g"][l, 1:4].T).transpose(0, 2, 1)),
                     wao=g["w_attn_out"][l], who=g["w_hyena_out"][l], wo=g["w_out"][l])
            if l % 2 == 0:
                i = l // 2
                m.update(w1=g["ffn_w1"][i], w3=g["ffn_w3"][i], w2=g["ffn_w2"][i])
            else:
                i = l // 2
                NE = c["NE"]
                es = np.zeros((NE, NE, 128), f32)
                for e in range(NE):
                    es[e, e, :] = 1.0
                m.update(w1=g["moe_w1"][i], w3=g["moe_w3"][i], w2=g["moe_w2"][i].reshape(NE * c["DFFE"], D),
                         wr=fm(g["moe_router"][i]), identf=ident_f, esel=es)
            maps.append(m)
        resC = run(build_C(c, l % 2 == 1, NTc), maps)
        xT = [resC[r]["xo"] for r in range(8)]
        if dbg is not None:
            dbg[("x", l)] = xT
    out = np.zeros((2, S, D), f32)
    for r, (b, q) in enumerate(cores):
        out[b, q * NL:(q + 1) * NL, :] = xT[r][:, 0:NL].T
    return out


def kernel(**inputs):
    return kernel_impl(inputs, CFG_FULL)
```
